# Optimizing a Trainium2 kernel written in Bass

```python
import jax, jax.numpy as jnp
from jax import lax
import numpy as np

D_MODEL = 4096
BATCH = 4
SEQ = 2048
DEPTH = 1

HEAD_DIM = 128
A_HEADS = 16
A_KV_HEADS = 4
WINDOW = 128
A_BLOCK = 128
ROT_DIM = HEAD_DIM // 4
ROPE_THETA = 500000.0
B_HEADS = 16
GRID_W = 64
NA_KH_MAX = 8
NA_KW = 16
MEM_LEN = 256
X_HEADS = 4
N_BRANCH = 2
N_EXPERTS = 16
EC_CAPACITY = 2
D_EXPERT = D_MODEL // 2
EPS = 1e-6
NEG = -1e30

QA_W = A_HEADS * HEAD_DIM
KVA_W = A_KV_HEADS * HEAD_DIM
QB_W = B_HEADS * HEAD_DIM
GATE_W = N_BRANCH * D_MODEL
IN_WIDTHS = [QA_W, KVA_W, KVA_W, QB_W, QB_W, QB_W, GATE_W]
IN_W = sum(IN_WIDTHS)
IN_SPLITS = [int(v) for v in np.cumsum(IN_WIDTHS)[:-1]]
X_W = X_HEADS * HEAD_DIM

kernel_name = 'hybrid_window_natten_ec_moe_encoder'


def rms_norm(x, g):
    xf = x.astype(jnp.float32)
    y = xf * lax.rsqrt(jnp.mean(xf * xf, axis=-1, keepdims=True) + EPS)
    return (y * g.astype(jnp.float32)).astype(x.dtype)


def partial_rotary(x, pos):
    half = ROT_DIM // 2
    inv = ROPE_THETA ** (-jnp.arange(half, dtype=jnp.float32) * 2.0 / ROT_DIM)
    ang = pos.astype(jnp.float32)[:, None] * inv[None, :]
    cos = jnp.cos(ang)[None, :, None, :]
    sin = jnp.sin(ang)[None, :, None, :]
    xr = x[..., :ROT_DIM].astype(jnp.float32)
    x1, x2 = xr[..., :half], xr[..., half:]
    rot = jnp.concatenate([x1 * cos - x2 * sin, x2 * cos + x1 * sin], axis=-1).astype(x.dtype)
    return jnp.concatenate([rot, x[..., ROT_DIM:]], axis=-1)


def window_gqa(q, k, v, sink):
    B, S, Hq, hd = q.shape
    Hkv = k.shape[2]
    G = Hq // Hkv
    nb = S // A_BLOCK
    pad = ((0, 0), (A_BLOCK, A_BLOCK), (0, 0), (0, 0))
    kp = jnp.pad(k, pad).reshape(B, nb + 2, A_BLOCK, Hkv, hd)
    vp = jnp.pad(v, pad).reshape(B, nb + 2, A_BLOCK, Hkv, hd)
    kb = jnp.concatenate([kp[:, :-2], kp[:, 1:-1], kp[:, 2:]], axis=2)
    vb = jnp.concatenate([vp[:, :-2], vp[:, 1:-1], vp[:, 2:]], axis=2)
    qb = q.reshape(B, nb, A_BLOCK, Hkv, G, hd)
    s = jnp.einsum('bnqkgd,bnckd->bnkgqc', qb, kb).astype(jnp.float32) * (hd ** -0.5)
    qi = jnp.arange(A_BLOCK)[:, None]
    kc = jnp.arange(3 * A_BLOCK)[None, :]
    in_band = jnp.abs(kc - A_BLOCK - qi) <= WINDOW
    key_abs = jnp.arange(nb)[:, None] * A_BLOCK - A_BLOCK + jnp.arange(3 * A_BLOCK)[None, :]
    key_ok = (key_abs >= 0) & (key_abs < S)
    mask = in_band[None] & key_ok[:, None, :]
    s = jnp.where(mask[None, :, None, None], s, NEG)
    sk = sink.astype(jnp.float32).reshape(Hkv, G)[None, None, :, :, None, None]
    sk = jnp.broadcast_to(sk, s.shape[:-1] + (1,))
    p = jax.nn.softmax(jnp.concatenate([s, sk], axis=-1), axis=-1)[..., :-1]
    o = jnp.einsum('bnkgqc,bnckd->bnqkgd', p.astype(v.dtype), vb)
    return o.reshape(B, S, Hq, hd)


def neighbourhood_attn(q, k, v, rpb):
    B, S, H, hd = q.shape
    rows = S // GRID_W
    kh = min(NA_KH_MAX, rows)
    kw = NA_KW
    r = jnp.arange(rows)
    c = jnp.arange(GRID_W)
    rs = jnp.clip(r - kh // 2, 0, rows - kh)
    cs = jnp.clip(c - kw // 2, 0, GRID_W - kw)
    krow = rs[:, None] + jnp.arange(kh)[None, :]
    tok = (krow[:, :, None] * GRID_W + c[None, None, :]).reshape(rows, kh * GRID_W)
    ks = jnp.take(k, tok, axis=1)
    vs = jnp.take(v, tok, axis=1)
    qr = q.reshape(B, rows, GRID_W, H, hd)
    s = jnp.einsum('brqhd,brkhd->bhrqk', qr, ks).astype(jnp.float32) * (hd ** -0.5)
    key_r = jnp.repeat(krow, GRID_W, axis=1)
    key_c = jnp.tile(c, kh)
    dr = key_r[:, None, :] - r[:, None, None]
    dc = key_c[None, None, :] - c[None, :, None]
    bias = rpb[:, dr + NA_KH_MAX - 1, jnp.clip(dc + kw - 1, 0, 2 * kw - 2)]
    col_ok = (key_c[None, :] >= cs[:, None]) & (key_c[None, :] < cs[:, None] + kw)
    s = jnp.where(col_ok[None, None, None], s + bias.astype(jnp.float32)[None], NEG)
    p = jax.nn.softmax(s, axis=-1)
    o = jnp.einsum('bhrqk,brkhd->brqhd', p.astype(v.dtype), vs)
    return o.reshape(B, S, H, hd)


def memory_cross_attn(h, m, wq, wk, wv, wo):
    B, S, _ = h.shape
    M = m.shape[1]
    q = (h @ wq).reshape(B, S, X_HEADS, HEAD_DIM)
    k = (m @ wk).reshape(B, M, X_HEADS, HEAD_DIM)
    v = (m @ wv).reshape(B, M, X_HEADS, HEAD_DIM)
    s = jnp.einsum('bshd,bmhd->bhsm', q, k).astype(jnp.float32) * (HEAD_DIM ** -0.5)
    p = jax.nn.softmax(s, axis=-1)
    o = jnp.einsum('bhsm,bmhd->bshd', p.astype(v.dtype), v).reshape(B, S, X_W)
    return o @ wo


def expert_choice_ffn(h, w_router, w_gate, w_up, w_down):
    B, S, D = h.shape
    cap = EC_CAPACITY * S // N_EXPERTS
    aff = jax.nn.softmax((h @ w_router).astype(jnp.float32), axis=-1)
    val, idx = lax.top_k(jnp.swapaxes(aff, 1, 2), cap)
    xg = jax.vmap(lambda hb, ib: hb[ib])(h, idx)
    a = jnp.einsum('becd,edf->becf', xg, w_gate)
    u = jnp.einsum('becd,edf->becf', xg, w_up)
    y = jnp.einsum('becf,efd->becd', jax.nn.silu(a) * u, w_down)
    y = y * val[..., None].astype(h.dtype)
    bidx = jnp.arange(B)[:, None, None]
    return jnp.zeros_like(h).at[bidx, idx].add(y)


def setup_inputs(seed: int = 0) -> dict:
    key = jax.random.key(seed)
    ks = jax.random.split(key, 24)
    f32 = jnp.float32
    L = DEPTH

    def nrm(k, shape, scale):
        return jax.random.normal(k, shape, f32) * scale

    def gain(k, shape):
        return 1.0 + 0.05 * jax.random.normal(k, shape, f32)

    return {
        'x': jax.random.normal(ks[0], (BATCH, SEQ, D_MODEL), f32),
        'mem': jax.random.normal(ks[1], (BATCH, MEM_LEN, D_MODEL), f32),
        'norm_mix': gain(ks[2], (L, D_MODEL)),
        'w_in': nrm(ks[3], (L, D_MODEL, IN_W), D_MODEL ** -0.5),
        'b_gate': nrm(ks[4], (L, GATE_W), 0.1),
        'sink': nrm(ks[5], (L, A_HEADS), 0.5),
        'rpb': nrm(ks[6], (L, B_HEADS, 2 * NA_KH_MAX - 1, 2 * NA_KW - 1), 0.2),
        'w_branch_a': nrm(ks[7], (L, QA_W, D_MODEL), QA_W ** -0.5),
        'w_branch_b': nrm(ks[8], (L, QB_W, D_MODEL), QB_W ** -0.5),
        'w_out': nrm(ks[9], (L, D_MODEL, D_MODEL), D_MODEL ** -0.5),
        'norm_cross': gain(ks[10], (L, D_MODEL)),
        'norm_mem': gain(ks[11], (L, D_MODEL)),
        'wq_x': nrm(ks[12], (L, D_MODEL, X_W), D_MODEL ** -0.5),
        'wk_x': nrm(ks[13], (L, D_MODEL, X_W), D_MODEL ** -0.5),
        'wv_x': nrm(ks[14], (L, D_MODEL, X_W), D_MODEL ** -0.5),
        'wo_x': nrm(ks[15], (L, X_W, D_MODEL), X_W ** -0.5),
        'norm_ffn': gain(ks[16], (L, D_MODEL)),
        'w_router': nrm(ks[17], (L, D_MODEL, N_EXPERTS), D_MODEL ** -0.5),
        'w_gate': nrm(ks[18], (L, N_EXPERTS, D_MODEL, D_EXPERT), D_MODEL ** -0.5),
        'w_up': nrm(ks[19], (L, N_EXPERTS, D_MODEL, D_EXPERT), D_MODEL ** -0.5),
        'w_down': nrm(ks[20], (L, N_EXPERTS, D_EXPERT, D_MODEL), D_EXPERT ** -0.5),
        'norm_final': gain(ks[21], (D_MODEL,)),
    }


def reference(x, mem, norm_mix, w_in, b_gate, sink, rpb, w_branch_a, w_branch_b, w_out,
              norm_cross, norm_mem, wq_x, wk_x, wv_x, wo_x, norm_ffn, w_router,
              w_gate, w_up, w_down, norm_final):
    B, S, D = x.shape
    pos = jnp.arange(S)
    for l in range(DEPTH):
        h = rms_norm(x, norm_mix[l])
        proj = h @ w_in[l]
        qa, ka, va, qb, kb, vb, gates = jnp.split(proj, IN_SPLITS, axis=-1)
        qa = partial_rotary(qa.reshape(B, S, A_HEADS, HEAD_DIM), pos)
        ka = partial_rotary(ka.reshape(B, S, A_KV_HEADS, HEAD_DIM), pos)
        va = va.reshape(B, S, A_KV_HEADS, HEAD_DIM)
        ya = window_gqa(qa, ka, va, sink[l]).reshape(B, S, QA_W) @ w_branch_a[l]
        yb = neighbourhood_attn(qb.reshape(B, S, B_HEADS, HEAD_DIM),
                                kb.reshape(B, S, B_HEADS, HEAD_DIM),
                                vb.reshape(B, S, B_HEADS, HEAD_DIM), rpb[l]).reshape(B, S, QB_W) @ w_branch_b[l]
        g = jax.nn.sigmoid((gates + b_gate[l]).astype(jnp.float32)).astype(x.dtype).reshape(B, S, N_BRANCH, D)
        x = x + (g[:, :, 0] * ya + g[:, :, 1] * yb) @ w_out[l]
        h = rms_norm(x, norm_cross[l])
        m = rms_norm(mem, norm_mem[l])
        x = x + memory_cross_attn(h, m, wq_x[l], wk_x[l], wv_x[l], wo_x[l])
        h = rms_norm(x, norm_ffn[l])
        x = x + expert_choice_ffn(h, w_router[l], w_gate[l], w_up[l], w_down[l])
    return rms_norm(x, norm_final)
```

```python
import numpy as np
import ml_dtypes
import concourse.bass as bass
import concourse.mybir as mybir
from concourse.bass_utils import run_bass_kernel_spmd

F32 = mybir.dt.float32
BF16 = mybir.dt.bfloat16
I32 = mybir.dt.int32
AF = mybir.ActivationFunctionType
ALU = mybir.AluOpType
AX = mybir.AxisListType

HD = 128
A_HEADS, A_KV, B_HEADS, X_HEADS = 16, 4, 16, 4
NEXP = 16
EPS = 1e-6
ROT = 32
THETA = 500000.0


class Cfg:
    def __init__(self, D=4096, S=2048, MEM=256, debug=False, stop_after=None):
        self.D = D
        self.S = S
        self.MEM = MEM
        self.DC = D // 128
        self.DE = D // 2
        self.FC = self.DE // 128
        self.CAP = 2 * S // NEXP
        self.QA = A_HEADS * HD
        self.KV = A_KV * HD
        self.QB = B_HEADS * HD
        self.INW = self.QA + 2 * self.KV + 3 * self.QB + 2 * D
        self.XW = X_HEADS * HD
        self.debug = debug
        self.stop_after = stop_after
        import os
        self.flags = set(os.environ.get("KFLAGS", "").split(","))


class Buf:
    __slots__ = ("name", "w", "r", "excl")

    def __init__(self, name="", excl=False):
        self.name = name
        self.w = None
        self.r = []
        self.excl = excl


class Ins:
    __slots__ = ("eng", "fn", "deps", "is_dma", "marked", "value", "sem", "prev", "virt")

    def __init__(self, eng, fn, is_dma):
        self.eng = eng
        self.fn = fn
        self.deps = []
        self.is_dma = is_dma
        self.marked = False
        self.value = 0
        self.sem = None
        self.prev = 0
        self.virt = False


ENGS = ["pe", "act", "dve", "pool", "sp"]
DMA_RING = 14


class Sched:
    def __init__(self, nc):
        self.nc = nc
        self.streams = {e: [] for e in ENGS}
        self.since_barrier = []

    def op(self, eng, fn, reads=(), writes=(), dma=False, strict=()):
        ins = Ins(eng, fn, dma)
        for b in strict:
            if b.w is not None and b.w is not ins:
                ins.deps.append(b.w)
                b.w.marked = True
        ex = [b for b in reads if b.excl]
        if ex:
            reads = [b for b in reads if not b.excl]
            writes = list(writes) + [b for b in ex if b not in writes]
        seen = set()
        cand = []
        for b in reads:
            if b.w is not None:
                cand.append((b.w, True))
        for b in writes:
            if b.w is not None:
                cand.append((b.w, True))
            cand.extend((r_, False) for r_ in b.r)
        for d, hard in cand:
            if d is ins:
                continue
            if (not d.is_dma) and (not dma) and d.eng == eng and (eng == "pe" or not hard):
                continue
            if id(d) in seen:
                continue
            seen.add(id(d))
            ins.deps.append(d)
            d.marked = True
        for b in reads:
            b.r.append(ins)
        for b in writes:
            b.w = ins
            b.r = []
        self.streams[eng].append(ins)
        self.since_barrier.append(ins)
        return ins

    def barrier(self):
        lasts = []
        for e in ENGS:
            comp = [i for i in self.streams[e] if not i.is_dma and not i.virt]
            if comp:
                lasts.append(comp[-1])
        dmas = [i for i in self.since_barrier if i.is_dma]
        self.since_barrier = []
        for e in ENGS:
            ins = Ins(e, lambda eng: None, False)
            ins.virt = True
            for d in lasts:
                if d.eng != e:
                    ins.deps.append(d)
                    d.marked = True
            for d in dmas:
                ins.deps.append(d)
            self.streams[e].append(ins)

    def dma(self, q, out, in_, reads=(), writes=()):
        return self.op(q, lambda e: e.dma_start(out=out, in_=in_), reads, writes, dma=True)

    def mm(self, out, lhsT, rhs, start, stop, reads=(), writes=()):
        return self.op("pe", lambda e: e.matmul(out, lhsT=lhsT, rhs=rhs, start=start, stop=stop), reads, writes)

    def tr(self, out, in_, ident, reads=(), writes=()):
        return self.op("pe", lambda e: e.transpose(out=out, in_=in_, identity=ident), reads, writes)

    def act(self, out, in_, func, reads=(), writes=(), bias=None, scale=1.0, accum_out=None):
        def f(e):
            kw = {}
            if bias is not None:
                kw["bias"] = bias
            if accum_out is not None:
                kw["accum_out"] = accum_out
            return e.activation(out=out, in_=in_, func=func, scale=scale, **kw)
        return self.op("act", f, reads, writes)

    def tt(self, eng, out, in0, in1, op, reads=(), writes=()):
        return self.op(eng, lambda e: e.tensor_tensor(out=out, in0=in0, in1=in1, op=op), reads, writes)

    def ts(self, eng, out, in0, s1, s2, op0, op1=None, reads=(), writes=(), accum_out=None, strict=()):
        def f(e):
            kw = {}
            if accum_out is not None:
                kw["accum_out"] = accum_out
            return e.tensor_scalar(out=out, in0=in0, scalar1=s1, scalar2=s2, op0=op0,
                                   op1=(op1 if op1 is not None else ALU.bypass), **kw)
        return self.op(eng, f, reads, writes, strict=strict)

    def copy(self, eng, out, in_, reads=(), writes=()):
        if eng == "act":
            return self.op(eng, lambda e: e.copy(out=out, in_=in_), reads, writes)
        return self.op(eng, lambda e: e.tensor_copy(out=out, in_=in_), reads, writes)

    def emit(self, final_reads):
        nc = self.nc
        self.op("sp", lambda e: None, reads=final_reads).virt = True
        sems = []

        def new_sem(name):
            sems.append(nc.alloc_semaphore(name))
            return len(sems) - 1

        comp_sem = {e: new_sem("c_" + e) for e in ENGS}
        ring = {e: [new_sem("d_%s_%d" % (e, k)) for k in range(DMA_RING)] for e in ("sp", "act", "pool")}
        for e in ENGS:
            cnt = 0
            dcount = 0
            rv = [0] * DMA_RING
            for ins in self.streams[e]:
                if ins.is_dma:
                    k = dcount % DMA_RING
                    dcount += 1
                    ins.sem = ring[e][k]
                    ins.prev = rv[k]
                    rv[k] += 16
                    ins.value = rv[k]
                elif ins.marked:
                    cnt += 1
                    ins.sem = comp_sem[e]
                    ins.value = cnt
        streams = self.streams

        def runner(name):
            def f(e):
                waited = {}
                for ins in streams[name]:
                    need = {}
                    for d in ins.deps:
                        if need.get(d.sem, 0) < d.value:
                            need[d.sem] = d.value
                    if ins.is_dma and ins.prev > 0 and need.get(ins.sem, 0) < ins.prev:
                        need[ins.sem] = ins.prev
                    for s, v in need.items():
                        if waited.get(s, 0) < v:
                            e.wait_ge(sems[s], v)
                            waited[s] = v
                    bi = ins.fn(e)
                    if bi is None:
                        assert not ins.marked
                        continue
                    if ins.is_dma:
                        bi.then_inc(sems[ins.sem], 16)
                    elif ins.marked:
                        bi.then_inc(sems[ins.sem], 1)
            return f

        with nc.Block() as block:
            block.tensor(runner("pe"))
            block.scalar(runner("act"))
            block.vector(runner("dve"))
            block.gpsimd(runner("pool"))
            block.sync(runner("sp"))


class Arena:
    LO = 16640
    HI = 226000

    def __init__(self, nc):
        self.nc = nc
        self.top = self.LO
        self.n = 0

    def alloc(self, shape, dtype, name="t"):
        nbytes = int(np.prod(shape[1:])) * mybir.dt.size(dtype)
        off = (self.top + 63) // 64 * 64
        assert off + nbytes <= self.HI, ("SBUF overflow", name, off, nbytes)
        self.top = off + nbytes
        self.n += 1
        return self.nc.alloc_sbuf_tensor_at("%s_%d" % (name, self.n), list(shape), dtype, offset=off)

    def mark(self):
        return self.top

    def release(self, m):
        self.top = m


def _na_index_tables():
    kc = np.arange(64)
    a = np.arange(2)
    key_r = np.repeat(a, 64)
    key_c = np.tile(kc, 2)
    q_r = np.repeat(a, 64)
    q_c = np.tile(kc, 2)
    cs = np.clip(q_c - 8, 0, 48)
    col_ok = (key_c[:, None] >= cs[None, :]) & (key_c[:, None] < cs[None, :] + 16)
    dc = key_c[:, None] - q_c[None, :]
    dci = np.clip(dc + 15, 0, 30)
    deltas = list(range(-3, 4))
    dri = np.zeros((7, 128, 128), np.int64)
    drv = np.zeros((7, 128, 128), bool)
    for i, dl in enumerate(deltas):
        dr = 2 * dl + key_r[:, None] - q_r[None, :]
        drv[i] = np.abs(dr) <= 7
        dri[i] = np.clip(dr + 7, 0, 14)
    return dri, dci, col_ok, drv


def make_consts(S):
    c = {}
    c["ident_f"] = np.eye(128, dtype=np.float32)
    k = np.arange(128)[:, None]
    q = np.arange(128)[None, :]
    c["maskL"] = (k >= q).astype(np.float32)
    c["maskR"] = (k <= q).astype(np.float32)
    dri, dci, col_ok, drv = _na_index_tables()
    nam = np.zeros((9, 128, 128), np.float32)
    a = np.repeat(np.arange(2), 64)
    for i, dl in enumerate(range(-3, 4)):
        nam[i] = (col_ok & drv[i]).astype(np.float32)
    for j, dl in ((7, -2), (8, 2)):
        dr = 2 * dl + a[:, None] - a[None, :]
        nam[j] = (col_ok & (dr >= -4) & (dr <= 3)).astype(np.float32)
    c["namask"] = nam
    half = ROT // 2
    inv = (THETA ** (-(np.arange(half, dtype=np.float32) * 2.0 / ROT))).astype(np.float32)
    ang = np.arange(S, dtype=np.float32)[None, :] * inv[:, None]
    cos = np.cos(ang).astype(np.float32)
    sin = np.sin(ang).astype(np.float32)
    c["cos"] = np.concatenate([cos, cos], 0)
    c["sin"] = np.concatenate([-sin, sin], 0)
    R = np.zeros((32, 32), np.float32)
    for m in range(16):
        R[m + 16, m] = 1.0
        R[m, m + 16] = 1.0
    c["rotR"] = R
    c["lt"] = (q < k).astype(np.float32)
    c["iota"] = np.tile(np.arange(256, dtype=np.float32)[None, :], (128, 1))
    c["tokidx"] = (np.arange(S // 128)[None, :] * 128 + np.arange(128)[:, None]).astype(np.float32)
    return c


def col_layout(v, nchunk):
    return np.ascontiguousarray(v.reshape(nchunk, 128).T)


def build(cfg):
    D, S, DC, MEM = cfg.D, cfg.S, cfg.DC, cfg.MEM
    NB = S // 128
    NT = S // 512
    nc = bass.Bass("TRN2", target_bir_lowering=False)
    dbg = cfg.debug

    def din(name, shape, dt=F32):
        return nc.dram_tensor(name, list(shape), dt, kind="ExternalInput").ap()

    def dscr(name, shape, dt):
        return nc.dram_tensor(name, list(shape), dt, kind=("ExternalOutput" if dbg else "Internal")).ap()

    x_d = din("x", [S, D])
    mem_d = din("mem", [MEM, D])
    gmix_d = din("gmix", [128, DC])
    gcross_d = din("gcross", [128, DC])
    gmem_d = din("gmem", [128, DC])
    gffn_d = din("gffn", [1, D])
    gfin_d = din("gfin", [1, D])
    w_in_d = din("w_in", [D, cfg.INW])
    bg_d = din("bgate", [128, 2 * DC])
    sink_d = din("sink", [1, A_HEADS])
    rpbg_d = din("rpbg", [B_HEADS, 7, 128, 128])
    wa_d = din("w_branch_a", [cfg.QA, D])
    wb_d = din("w_branch_b", [cfg.QB, D])
    wout_d = din("w_out", [D, D])
    wq_d = din("wq_x", [D, cfg.XW])
    wk_d = din("wk_x", [D, cfg.XW])
    wv_d = din("wv_x", [D, cfg.XW])
    wo_d = din("wo_x", [cfg.XW, D])
    wr_d = din("w_router", [D, NEXP])
    wg_d = din("w_gate", [NEXP, D, cfg.DE])
    wu_d = din("w_up", [NEXP, D, cfg.DE])
    wd_d = din("w_down", [NEXP, cfg.DE, D])
    c_ident = din("c_ident", [128, 128])
    c_maskL = din("c_maskL", [128, 128])
    c_maskR = din("c_maskR", [128, 128])
    c_namask = din("c_namask", [9, 128, 128])
    c_cos = din("c_cos", [32, S])
    c_sin = din("c_sin", [32, S])
    c_rotR = din("c_rotR", [32, 32])
    c_iota = din("c_iota", [128, 256])
    c_lt = din("c_lt", [128, 128])
    c_tok = din("c_tok", [128, NB])
    out_d = nc.dram_tensor("out", [S, D], F32, kind="ExternalOutput").ap()

    qaT_d = dscr("s_qaT", [A_HEADS, 128, S], BF16)
    kaT_d = dscr("s_kaT", [A_KV, 128, S], BF16)
    va_d = dscr("s_va", [S, cfg.KV], BF16)
    qbT_d = dscr("s_qbT", [B_HEADS, 128, S], BF16)
    kbT_d = dscr("s_kbT", [B_HEADS, 128, S], BF16)
    vb_d = dscr("s_vb", [S, cfg.QB], BF16)
    gT_d = dscr("s_gT", [2 * DC, 128, S], BF16)
    oaT_d = dscr("s_oaT", [A_HEADS, 128, S], BF16)
    obT_d = dscr("s_obT", [B_HEADS, 128, S], BF16)
    mgT_d = dscr("s_mgT", [DC, 128, S], BF16)
    x1_d = dscr("s_x1", [S, D], F32)
    acc_d = dscr("s_acc", [S, D], F32)
    h3_d = dscr("s_h3", [S, D], BF16)
    affT_d = dscr("s_affT", [NEXP, S], F32)

    sch = Sched(nc)
    A = Arena(nc)

    banks = [nc.alloc_psum_tensor("psb%d" % i, [128, 512], F32) for i in range(8)]
    bankB = [Buf("psb%d" % i, excl=True) for i in range(8)]

    def pbf(i):
        return banks[i][:].bitcast(BF16)

    ident_f = A.alloc([128, 128], F32, "identf")
    ident_b = A.alloc([128, 128], BF16, "identb")
    ones_b = A.alloc([128, 128], BF16, "onesb")
    eps_t = A.alloc([128, 1], F32, "eps")
    constB = Buf("const")
    sch.dma("sp", ident_f[:], c_ident, writes=[constB])
    sch.copy("dve", ident_b[:], ident_f[:], reads=[constB], writes=[constB])
    sch.op("dve", lambda e: e.memset(ones_b[:], 1.0), writes=[constB])
    sch.op("dve", lambda e: e.memset(eps_t[:], EPS), writes=[constB])
    sch.barrier()
    base_mark = A.mark()

    def norm_fm(src_d, T, gcol_d, hT, hTB):
        m = A.mark()
        nblk = T // 128
        gcol = A.alloc([128, DC], F32, "gcol")
        ss = A.alloc([128, nblk], F32, "ss")
        sd = A.alloc([128, nblk], F32, "sd")
        rstd = A.alloc([128, nblk], F32, "rstd")
        xr = [A.alloc([128, D], F32, "xr") for _ in range(2)]
        xs = [A.alloc([128, D], BF16, "xs") for _ in range(2)]
        xrB = [Buf("xr0"), Buf("xr1")]
        xsB = [Buf("xs0"), Buf("xs1")]
        gB = Buf("gcol")
        stB = [Buf("st%d" % i) for i in range(nblk)]
        sch.dma("sp", gcol[:], gcol_d, writes=[gB])
        G = min(8, DC)
        pbank = [0, 1]
        step = 0
        for i in range(nblk):
            s = i % 2
            sch.dma("sp", xr[s][:], src_d[i * 128:(i + 1) * 128, :], writes=[xrB[s]])
            sch.op("dve", (lambda xs_=xs[s], xr_=xr[s], i_=i: lambda e: e.scalar_tensor_tensor(
                out=xs_[:], in0=xr_[:], scalar=1.0, in1=xr_[:], op0=ALU.mult, op1=ALU.mult,
                accum_out=ss[:, i_:i_ + 1]))(), reads=[xrB[s]], writes=[xsB[s], stB[i]])
            sch.act(sd[:, i:i + 1], ss[:, i:i + 1], AF.Sqrt, reads=[stB[i]], writes=[stB[i]],
                    bias=eps_t[:], scale=1.0 / D)
            sch.op("dve", (lambda i_=i: lambda e: e.reciprocal(out=rstd[:, i_:i_ + 1], in_=sd[:, i_:i_ + 1]))(),
                   reads=[stB[i]], writes=[stB[i]])
            sch.ts("pool", xs[s][:], xr[s][:], rstd[:, i:i + 1], None, ALU.mult,
                   reads=[xrB[s], stB[i]], writes=[xsB[s]])
            for c0 in range(0, DC, G):
                pb = pbank[step % 2]
                step += 1
                pv = pbf(pb).rearrange("p (j t) -> p j t", t=128)
                for j in range(G):
                    c = c0 + j
                    sch.tr(pv[:, j, :], xs[s][:, c * 128:(c + 1) * 128], ident_b[:],
                           reads=[xsB[s]], writes=[bankB[pb]])
                sch.tt("dve", hT[:, c0:c0 + G, i * 128:(i + 1) * 128], pv[:, 0:G, :],
                       gcol[:, c0:c0 + G].unsqueeze(2).to_broadcast([128, G, 128]), ALU.mult,
                       reads=[bankB[pb], gB], writes=[hTB[i]])
        sch.barrier()
        A.release(m)

    hT = A.alloc([128, DC, S], BF16, "hT")
    hTB = [Buf("hT%d" % i) for i in range(NB)]
    m_h = A.mark()
    norm_fm(x_d, S, gmix_d, hT, hTB)
    sch.barrier()
    if cfg.stop_after == "N1":
        hT_dbg = dscr("s_hT", [128, DC, S], BF16)
        dB = Buf("dbg")
        sch.dma("sp", hT_dbg, hT[:], reads=hTB, writes=[dB])
        sch.emit([dB])
        return nc

    WS = 256
    NWR = 2
    wring = [A.alloc([128, DC, WS], BF16, "wr") for _ in range(NWR)]
    wrB = [Buf("wr%d" % i) for i in range(NWR)]
    cos_t = A.alloc([32, S], F32, "cos")
    sin_t = A.alloc([32, S], F32, "sin")
    rotR = A.alloc([32, 32], F32, "rotR")
    bgc = A.alloc([128, 2 * DC], F32, "bgc")
    tabB = Buf("tabs")
    sch.dma("sp", cos_t[:], c_cos, writes=[tabB])
    sch.dma("sp", sin_t[:], c_sin, writes=[tabB])
    sch.dma("sp", rotR[:], c_rotR, writes=[tabB])
    sch.dma("sp", bgc[:], bg_d, writes=[tabB])
    stg = [A.alloc([128, 512], BF16, "stg") for _ in range(4)]
    stgB = [Buf("stg%d" % i) for i in range(4)]
    x32 = [A.alloc([32, 512], F32, "x32") for _ in range(2)]
    x32B = [Buf("x32a"), Buf("x32b")]
    t1 = [A.alloc([32, 512], F32, "t1") for _ in range(2)]
    t2 = [A.alloc([32, 512], F32, "t2") for _ in range(2)]

    scrB = {}

    def sB(key):
        if key not in scrB:
            scrB[key] = Buf(str(key))
        return scrB[key]

    nslots = cfg.INW // WS
    va0, va1 = cfg.QA + cfg.KV, cfg.QA + 2 * cfg.KV
    vb0, vb1 = va1 + 2 * cfg.QB, va1 + 3 * cfg.QB
    g0 = vb1
    cnt = {"stg": 0, "ps": 0, "rot": 0, "ev": 0}

    def next_stg():
        k = cnt["stg"] % 4
        cnt["stg"] += 1
        return k

    def fm_unit(wslot, wB, sub, col):
        if col < cfg.QA:
            kind, idx, dst = "rot", col // 128, qaT_d
        elif col < va0:
            kind, idx, dst = "rot", (col - cfg.QA) // 128, kaT_d
        elif col < va1 + cfg.QB:
            kind, idx, dst = "plain", (col - va1) // 128, qbT_d
        elif col < vb0:
            kind, idx, dst = "plain", (col - va1 - cfg.QB) // 128, kbT_d
        else:
            kind, idx, dst = "gate", (col - g0) // 128, gT_d
        if "norot" in cfg.flags and kind == "rot":
            kind = "plain"
        if "nogate" in cfg.flags and kind == "gate":
            kind = "plain"
        for tb in range(NT):
            pb = 2 + (cnt["ps"] % 4)
            cnt["ps"] += 1
            ps = banks[pb]
            for c in range(DC):
                sch.mm(ps[:, :], wslot[:, c, sub * 128:(sub + 1) * 128], hT[:, c, tb * 512:(tb + 1) * 512],
                       c == 0, c == DC - 1, reads=[wB] + hTB[4 * tb:4 * tb + 4], writes=[bankB[pb]])
            k = next_stg()
            dkey = (id(dst), idx, tb)
            if kind == "plain":
                ev = "act" if cnt["ev"] % 2 == 0 else "dve"
                cnt["ev"] += 1
                sch.copy(ev, stg[k][:], ps[:, :], reads=[bankB[pb]], writes=[stgB[k]])
            elif kind == "gate":
                sch.act(stg[k][:], ps[:, :], AF.Sigmoid, reads=[bankB[pb], tabB], writes=[stgB[k]],
                        bias=bgc[:, idx:idx + 1])
            else:
                r = cnt["rot"] % 2
                cnt["rot"] += 1
                tsl = slice(tb * 512, (tb + 1) * 512)
                sch.copy("dve", x32[r][:], ps[0:32, :], reads=[bankB[pb]], writes=[x32B[r]])
                sch.copy("act", stg[k][:], ps[:, :], reads=[bankB[pb]], writes=[stgB[k]])
                pr = r
                sch.mm(banks[pr][0:32, :], rotR[:], x32[r][:], True, True, reads=[x32B[r], tabB], writes=[bankB[pr]])
                sch.tt("dve", t1[r][:], x32[r][:], cos_t[:, tsl], ALU.mult, reads=[x32B[r], tabB], writes=[x32B[r]])
                sch.tt("dve", t2[r][:], banks[pr][0:32, :], sin_t[:, tsl], ALU.mult, reads=[bankB[pr], tabB],
                       writes=[x32B[r]])
                sch.tt("dve", stg[k][0:32, :], t1[r][:], t2[r][:], ALU.add, reads=[x32B[r]], writes=[stgB[k]])
            sch.dma("sp", dst[idx, :, tb * 512:(tb + 1) * 512], stg[k][:], reads=[stgB[k]], writes=[sB(dkey)])

    def tm_slot(wslot, wB, col0):
        if col0 < va1:
            dst, dcol, key = va_d, col0 - va0, "va"
        else:
            dst, dcol, key = vb_d, col0 - vb0, "vb"
        for i in range(NB):
            pb = 2 + (cnt["ps"] % 4)
            cnt["ps"] += 1
            ps = banks[pb]
            for c in range(DC):
                sch.mm(ps[:, 0:WS], hT[:, c, i * 128:(i + 1) * 128], wslot[:, c, :], c == 0, c == DC - 1,
                       reads=[wB, hTB[i]], writes=[bankB[pb]])
            k = next_stg()
            ev = "act" if cnt["ev"] % 2 == 0 else "dve"
            cnt["ev"] += 1
            sch.copy(ev, stg[k][:, 0:WS], ps[:, 0:WS], reads=[bankB[pb]], writes=[stgB[k]])
            sch.dma("sp", dst[i * 128:(i + 1) * 128, dcol:dcol + WS], stg[k][:, 0:WS], reads=[stgB[k]],
                    writes=[sB((key, i))])

    def load_w(ws):
        s = ws % NWR
        col0 = ws * WS
        sch.dma("pool", wring[s][:], w_in_d[:, col0:col0 + WS].rearrange("(c p) n -> p c n", p=128),
                writes=[wrB[s]])

    PF = NWR - 1
    for ws in range(min(PF, nslots)):
        load_w(ws)
    for ws in range(nslots):
        if ws + PF < nslots:
            load_w(ws + PF)
        s = ws % NWR
        col0 = ws * WS
        if (va0 <= col0 < va1) or (vb0 <= col0 < vb1):
            tm_slot(wring[s], wrB[s], col0)
        else:
            fm_unit(wring[s], wrB[s], 0, col0)
            fm_unit(wring[s], wrB[s], 1, col0 + 128)
    sch.barrier()
    A.release(base_mark)


    SCALE = float(HD) ** -0.5

    def grp(lst):
        runs = []
        for slot, pos in lst:
            if runs and runs[-1][1] + runs[-1][2] == pos and runs[-1][0] + runs[-1][2] == slot:
                runs[-1][2] += 1
            else:
                runs.append([slot, pos, 1])
        return runs

    def stage_A1():
        m0 = A.mark()
        mk32 = A.alloc([128, 2, 128], F32, "mk32")
        mkb = A.alloc([128, 2, 128], BF16, "mkb")
        sink_t = A.alloc([128, A_HEADS], F32, "sink")
        sinkexp = A.alloc([128, A_HEADS], F32, "sinkexp")
        cB = Buf("a1c")
        sch.dma("sp", mk32[:, 0, :], c_maskL, writes=[cB])
        sch.dma("sp", mk32[:, 1, :], c_maskR, writes=[cB])
        sch.dma("sp", sink_t[:], sink_d.partition_broadcast(128), writes=[cB])
        sch.copy("dve", mkb[:], mk32[:], reads=[cB], writes=[cB])
        sch.act(sinkexp[:], sink_t[:], AF.Exp, reads=[cB], writes=[cB])
        kT = [A.alloc([128, S], BF16, "kT") for _ in range(2)]
        vv = [A.alloc([128, NB, 128], BF16, "vv") for _ in range(2)]
        qT = [A.alloc([128, 4, S], BF16, "qT") for _ in range(2)]
        oacc = [A.alloc([128, 4, S], BF16, "oacc") for _ in range(2)]
        inB = [Buf("a1in0"), Buf("a1in1")]
        oaB = [Buf("oacc0"), Buf("oacc1")]
        pT = [A.alloc([128, 3, 512], BF16, "pT") for _ in range(2)]
        pTB = [Buf("pT0"), Buf("pT1")]
        rec = [A.alloc([128, 512], F32, "rec") for _ in range(2)]
        recB = [Buf("rec0"), Buf("rec1")]
        deps_in = [sB(k) for k in list(scrB.keys())]
        it = 0
        for g in range(A_KV):
            s = g % 2
            sch.dma("sp", kT[s][:], kaT_d[g], reads=deps_in, writes=[inB[s]])
            sch.dma("sp", vv[s][:], va_d[:, g * 128:(g + 1) * 128].rearrange("(j p) d -> p j d", p=128),
                    reads=deps_in, writes=[inB[s]])
            for hh in range(4):
                sch.dma("sp", qT[s][:, hh, :], qaT_d[4 * g + hh], reads=deps_in, writes=[inB[s]])
            for n in range(NB):
                js = [j for j in (n - 1, n, n + 1) if 0 <= j < NB]
                par = it % 2
                it += 1
                sb = [0, 1, 2] if par == 0 else [3, 4, 5]
                for sl, j in enumerate(js):
                    sch.mm(banks[sb[sl]][:, :].rearrange("p (h q) -> p h q", h=4), kT[s][:, j * 128:(j + 1) * 128],
                           qT[s][:, :, n * 128:(n + 1) * 128], True, True, reads=[inB[s]], writes=[bankB[sb[sl]]])
                for sl, j in enumerate(js):
                    sch.act(pT[par][:, sl, :], banks[sb[sl]][:, :], AF.Exp, reads=[bankB[sb[sl]]], writes=[pTB[par]],
                            scale=SCALE)
                    if j != n:
                        mi = 0 if j == n - 1 else 1
                        pv = pT[par][:, sl, :].rearrange("p (h q) -> p h q", h=4)
                        sch.tt("dve", pv, pv, mkb[:, mi:mi + 1, :].to_broadcast([128, 4, 128]), ALU.mult,
                               reads=[cB], writes=[pTB[par]])
                for sl, j in enumerate(js):
                    sch.mm(banks[6][:, :], vv[s][:, j, :], pT[par][:, sl, :], sl == 0, sl == len(js) - 1,
                           reads=[inB[s], pTB[par]], writes=[bankB[6]])
                for sl, j in enumerate(js):
                    sch.mm(banks[7][:, :], ones_b[:], pT[par][:, sl, :], sl == 0, sl == len(js) - 1,
                           reads=[pTB[par]], writes=[bankB[7]])
                r3 = rec[par][:].rearrange("p (h q) -> p h q", h=4)
                sch.tt("dve", r3, banks[7][:, :].rearrange("p (h q) -> p h q", h=4),
                       sinkexp[:, 4 * g:4 * g + 4].unsqueeze(2).to_broadcast([128, 4, 128]), ALU.add,
                       reads=[bankB[7], cB], writes=[recB[par]])
                sch.op("dve", (lambda r_=rec[par]: lambda e: e.reciprocal(out=r_[:], in_=r_[:]))(),
                       reads=[], writes=[recB[par]])
                sch.tt("dve", oacc[s][:, :, n * 128:(n + 1) * 128], banks[6][:, :].rearrange("p (h q) -> p h q", h=4),
                       r3, ALU.mult, reads=[bankB[6], recB[par]], writes=[oaB[s]])
            for hh in range(4):
                sch.dma("sp", oaT_d[4 * g + hh], oacc[s][:, hh, :], reads=[oaB[s]], writes=[sB(("oaT", 4 * g + hh))])
        sch.barrier()
        A.release(m0)

    stage_A1()
    if cfg.stop_after == "A1":
        sch.emit(list(scrB.values()))
        return nc

    TPOS = {("int", -2): 0, ("full", -1): 1, ("full", 0): 2, ("full", 1): 3, ("int", 2): 4,
            ("full", -3): 5, ("full", -2): 6, ("full", 2): 7, ("full", 3): 8}

    def na_blocks(m):
        nm = NB
        if 2 <= m <= nm - 3:
            return [(m + d, TPOS[("int", d)] if abs(d) == 2 else TPOS[("full", d)]) for d in range(-2, 3)]
        if m < 2:
            js = list(range(0, 4))
        else:
            js = list(range(nm - 4, nm))
        return [(j, TPOS[("full", j - m)]) for j in js]

    def stage_A2():
        m0 = A.mark()
        Tt = A.alloc([128, B_HEADS, 9, 128], BF16, "Tt")
        nam = A.alloc([128, 9, 128], F32, "nam")
        rtmp = [A.alloc([128, 7, 128], F32, "rtmp") for _ in range(2)]
        rB = [Buf("rtmp0"), Buf("rtmp1")]
        TB = Buf("Tt")
        sch.dma("sp", nam[:], c_namask.rearrange("d k q -> k d q"), writes=[TB])
        for h in range(B_HEADS):
            s = h % 2
            sch.dma("sp", rtmp[s][:], rpbg_d[h].rearrange("d k q -> k d q"), writes=[rB[s]])
            sch.act(rtmp[s][:], rtmp[s][:], AF.Exp, reads=[], writes=[rB[s]])
            for (kind, d), pos in TPOS.items():
                src = d + 3
                mi = src if kind == "full" else (7 if d == -2 else 8)
                sch.tt("dve", Tt[:, h, pos, :], rtmp[s][:, src, :], nam[:, mi, :], ALU.mult,
                       reads=[rB[s]], writes=[TB])
        qT = [A.alloc([128, S], BF16, "qT") for _ in range(2)]
        kT = [A.alloc([128, S], BF16, "kT") for _ in range(2)]
        vv = [A.alloc([128, NB, 128], BF16, "vv") for _ in range(2)]
        oacc = [A.alloc([128, S], BF16, "oacc") for _ in range(2)]
        inB = [Buf("a2in0"), Buf("a2in1")]
        oaB = [Buf("a2o0"), Buf("a2o1")]
        pT = [A.alloc([128, 5, 128], BF16, "pT") for _ in range(2)]
        pTB = [Buf("a2pT0"), Buf("a2pT1")]
        rec = [A.alloc([128, 128], F32, "rec") for _ in range(2)]
        recB = [Buf("a2rec0"), Buf("a2rec1")]
        deps_in = [sB(k) for k in list(scrB.keys())]
        it = 0
        for h in range(B_HEADS):
            s = h % 2
            sch.dma("sp", qT[s][:], qbT_d[h], reads=deps_in, writes=[inB[s]])
            sch.dma("sp", kT[s][:], kbT_d[h], reads=deps_in, writes=[inB[s]])
            sch.dma("sp", vv[s][:], vb_d[:, h * 128:(h + 1) * 128].rearrange("(j p) d -> p j d", p=128),
                    reads=deps_in, writes=[inB[s]])
            for m in range(NB):
                blks = na_blocks(m)
                par = it % 2
                it += 1
                sbk = [0, 1] if par == 0 else [2, 3]
                ob, db = (4, 5) if par == 0 else (6, 7)
                for jj, (j, pos) in enumerate(blks):
                    bk = sbk[0] if jj < 4 else sbk[1]
                    col = (jj % 4) * 128
                    sch.mm(banks[bk][:, col:col + 128], kT[s][:, j * 128:(j + 1) * 128], qT[s][:, m * 128:(m + 1) * 128],
                           True, True, reads=[inB[s]], writes=[bankB[bk]])
                n4 = min(4, len(blks))
                sch.act(pT[par][:, 0:n4, :], banks[sbk[0]][:, 0:n4 * 128].rearrange("p (j q) -> p j q", q=128), AF.Exp,
                        reads=[bankB[sbk[0]]], writes=[pTB[par]], scale=SCALE)
                if len(blks) == 5:
                    sch.act(pT[par][:, 4, :], banks[sbk[1]][:, 0:128], AF.Exp, reads=[bankB[sbk[1]]], writes=[pTB[par]],
                            scale=SCALE)
                for slot, pos, cntr in grp([(jj, pos) for jj, (j, pos) in enumerate(blks)]):
                    sch.tt("dve", pT[par][:, slot:slot + cntr, :], pT[par][:, slot:slot + cntr, :],
                           Tt[:, h, pos:pos + cntr, :], ALU.mult, reads=[TB], writes=[pTB[par]])
                for jj, (j, pos) in enumerate(blks):
                    sch.mm(banks[ob][:, 0:128], vv[s][:, j, :], pT[par][:, jj, :], jj == 0, jj == len(blks) - 1,
                           reads=[inB[s], pTB[par]], writes=[bankB[ob]])
                for jj, (j, pos) in enumerate(blks):
                    sch.mm(banks[db][:, 0:128], ones_b[:], pT[par][:, jj, :], jj == 0, jj == len(blks) - 1,
                           reads=[pTB[par]], writes=[bankB[db]])
                sch.op("dve", (lambda r_=rec[par], d_=banks[db]: lambda e: e.reciprocal(out=r_[:], in_=d_[:, 0:128]))(),
                       reads=[bankB[db]], writes=[recB[par]])
                sch.tt("dve", oacc[s][:, m * 128:(m + 1) * 128], banks[ob][:, 0:128], rec[par][:], ALU.mult,
                       reads=[bankB[ob], recB[par]], writes=[oaB[s]])
            sch.dma("sp", obT_d[h], oacc[s][:], reads=[oaB[s]], writes=[sB(("obT", h))])
        sch.barrier()
        A.release(m0)

    stage_A2()
    if cfg.stop_after == "A2":
        sch.emit(list(scrB.values()))
        return nc


    def stage_B1():
        m0 = A.mark()
        oaT = A.alloc([128, A_HEADS, S], BF16, "oaT")
        obT = A.alloc([128, B_HEADS, S], BF16, "obT")
        oB = Buf("b1o")
        deps_in = [sB(k) for k in list(scrB.keys())]
        for h in range(A_HEADS):
            sch.dma("sp", oaT[:, h, :], oaT_d[h], reads=deps_in, writes=[oB])
            sch.dma("sp", obT[:, h, :], obT_d[h], reads=deps_in, writes=[oB])
        wa = [A.alloc([128, A_HEADS, 128], BF16, "wa") for _ in range(2)]
        wb = [A.alloc([128, B_HEADS, 128], BF16, "wb") for _ in range(2)]
        wB_ = [Buf("b1w0"), Buf("b1w1")]
        gt = [A.alloc([128, 2, S], BF16, "gt") for _ in range(2)]
        gB = [Buf("b1g0"), Buf("b1g1")]
        tmp = [A.alloc([128, 2, 512], F32, "tmp") for _ in range(2)]
        tB = [Buf("b1t0"), Buf("b1t1")]
        mg = [A.alloc([128, S], BF16, "mg") for _ in range(2)]
        mB = [Buf("b1m0"), Buf("b1m1")]

        def load(c):
            s = c % 2
            sch.dma("pool", wa[s][:], wa_d[:, c * 128:(c + 1) * 128].rearrange("(h p) n -> p h n", p=128), writes=[wB_[s]])
            sch.dma("pool", wb[s][:], wb_d[:, c * 128:(c + 1) * 128].rearrange("(h p) n -> p h n", p=128), writes=[wB_[s]])
            sch.dma("sp", gt[s][:, 0, :], gT_d[c], reads=deps_in, writes=[gB[s]])
            sch.dma("sp", gt[s][:, 1, :], gT_d[DC + c], reads=deps_in, writes=[gB[s]])

        load(0)
        it = 0
        for c in range(DC):
            if c + 1 < DC:
                load(c + 1)
            s = c % 2
            for tb in range(NT):
                par = it % 2
                it += 1
                ba, bb = (0, 1) if par == 0 else (2, 3)
                tsl = slice(tb * 512, (tb + 1) * 512)
                for h in range(A_HEADS):
                    sch.mm(banks[ba][:, :], wa[s][:, h, :], oaT[:, h, tsl], h == 0, h == A_HEADS - 1,
                           reads=[wB_[s], oB], writes=[bankB[ba]])
                for h in range(B_HEADS):
                    sch.mm(banks[bb][:, :], wb[s][:, h, :], obT[:, h, tsl], h == 0, h == B_HEADS - 1,
                           reads=[wB_[s], oB], writes=[bankB[bb]])
                sch.tt("dve", tmp[par][:, 0, :], banks[ba][:, :], gt[s][:, 0, tsl], ALU.mult,
                       reads=[bankB[ba], gB[s]], writes=[tB[par]])
                sch.tt("dve", tmp[par][:, 1, :], banks[bb][:, :], gt[s][:, 1, tsl], ALU.mult,
                       reads=[bankB[bb], gB[s]], writes=[tB[par]])
                sch.tt("pool", mg[s][:, tsl], tmp[par][:, 0, :], tmp[par][:, 1, :], ALU.add,
                       reads=[tB[par]], writes=[mB[s]])
            sch.dma("sp", mgT_d[c], mg[s][:], reads=[mB[s]], writes=[sB(("mgT", c))])
        sch.barrier()
        A.release(m0)

    stage_B1()
    if cfg.stop_after == "B1":
        sch.emit(list(scrB.values()))
        return nc

    def gemm_tm_res(actT, KC, w_d, res_d, res_deps, out_d, out_key):
        actB = res_deps[0]
        m0 = A.mark()
        CW = 256
        NG = 2
        wr = [A.alloc([128, KC, CW], BF16, "gw") for _ in range(NG)]
        wB_ = [Buf("gw%d" % i) for i in range(NG)]
        xr = [A.alloc([128, CW], F32, "gx") for _ in range(4)]
        xB = [Buf("gx%d" % i) for i in range(4)]
        orr = [A.alloc([128, CW], F32, "go") for _ in range(4)]
        oB = [Buf("go%d" % i) for i in range(4)]
        ncb = D // CW

        def loadw(cb):
            sch.dma("pool", wr[cb % NG][:], w_d[:, cb * CW:(cb + 1) * CW].rearrange("(c p) n -> p c n", p=128),
                    writes=[wB_[cb % NG]])

        loadw(0)
        it = 0
        for cb in range(ncb):
            if cb + 1 < ncb:
                loadw(cb + 1)
            ws = cb % NG
            for i in range(NB):
                k = it % 4
                it += 1
                sch.dma("sp", xr[k][:], res_d[i * 128:(i + 1) * 128, cb * CW:(cb + 1) * CW], reads=res_deps[1],
                        writes=[xB[k]])
                for c in range(KC):
                    sch.mm(banks[k][:, 0:CW], actT[:, c, i * 128:(i + 1) * 128], wr[ws][:, c, :], c == 0, c == KC - 1,
                           reads=[wB_[ws]] + actB, writes=[bankB[k]])
                sch.tt("dve", orr[k][:], banks[k][:, 0:CW], xr[k][:], ALU.add, reads=[bankB[k], xB[k]], writes=[oB[k]])
                sch.dma("sp", out_d[i * 128:(i + 1) * 128, cb * CW:(cb + 1) * CW], orr[k][:], reads=[oB[k]],
                        writes=[sB((out_key, i))])
        A.release(m0)

    NMB = MEM // 128
    k2T = A.alloc([128, X_HEADS, MEM], BF16, "k2T")
    v2 = A.alloc([128, NMB, cfg.XW], BF16, "v2")
    o2T = A.alloc([128, X_HEADS, S], BF16, "o2T")
    pre_act_mark = A.mark()
    actT = A.alloc([128, DC, S], BF16, "actT")
    actTB = [Buf("actT%d" % i) for i in range(NB)]
    act_mark = A.mark()
    deps_in = [sB(k) for k in list(scrB.keys())]
    for c in range(DC):
        sch.dma("sp", actT[:, c, :], mgT_d[c], reads=deps_in, writes=actTB)
    gemm_tm_res(actT, DC, wout_d, x_d, [actTB, []], x1_d, "x1")
    sch.barrier()
    if cfg.stop_after == "O1":
        sch.emit(list(scrB.values()))
        return nc


    A.release(pre_act_mark)
    kvB = Buf("kv2")

    def stage_M():
        m0 = A.mark()
        mT = A.alloc([128, DC, MEM], BF16, "mT")
        mTB = [Buf("mT%d" % i) for i in range(NMB)]
        norm_fm(mem_d, MEM, gmem_d, mT, mTB)
        wk = [A.alloc([128, DC, 128], BF16, "wk") for _ in range(2)]
        wkB = [Buf("wk0"), Buf("wk1")]
        wv = [A.alloc([128, DC, 256], BF16, "wv") for _ in range(2)]
        wvB = [Buf("wv0"), Buf("wv1")]
        for hh in range(X_HEADS):
            s = hh % 2
            sch.dma("pool", wk[s][:], wk_d[:, hh * 128:(hh + 1) * 128].rearrange("(c p) n -> p c n", p=128), writes=[wkB[s]])
            for c in range(DC):
                sch.mm(banks[s][:, 0:MEM], wk[s][:, c, :], mT[:, c, :], c == 0, c == DC - 1,
                       reads=[wkB[s]] + mTB, writes=[bankB[s]])
            sch.copy("act", k2T[:, hh, :], banks[s][:, 0:MEM], reads=[bankB[s]], writes=[kvB])
        for cb in range(cfg.XW // 256):
            s = cb % 2
            sch.dma("pool", wv[s][:], wv_d[:, cb * 256:(cb + 1) * 256].rearrange("(c p) n -> p c n", p=128), writes=[wvB[s]])
            for blk in range(NMB):
                pb = 2 + (cb * NMB + blk) % 2
                for c in range(DC):
                    sch.mm(banks[pb][:, 0:256], mT[:, c, blk * 128:(blk + 1) * 128], wv[s][:, c, :], c == 0, c == DC - 1,
                           reads=[wvB[s], mTB[blk]], writes=[bankB[pb]])
                sch.copy("dve", v2[:, blk, cb * 256:(cb + 1) * 256], banks[pb][:, 0:256], reads=[bankB[pb]], writes=[kvB])
        sch.barrier()
        A.release(m0)

    stage_M()
    A.release(act_mark)

    norm_fm(x1_d, S, gcross_d, actT, actTB)
    sch.barrier()

    def stage_X():
        m0 = A.mark()
        wq = [A.alloc([128, DC, 128], BF16, "wq") for _ in range(2)]
        wqB = [Buf("wq0"), Buf("wq1")]
        q2s = [A.alloc([128, 512], BF16, "q2s") for _ in range(2)]
        q2B = [Buf("q2s0"), Buf("q2s1")]
        pT = [A.alloc([128, NMB, 512], BF16, "xpT") for _ in range(2)]
        pTB = [Buf("xpT0"), Buf("xpT1")]
        rec = [A.alloc([128, 512], F32, "xrec") for _ in range(2)]
        recB = [Buf("xrec0"), Buf("xrec1")]
        o2B = Buf("o2T")
        it = 0
        for hh in range(X_HEADS):
            s = hh % 2
            sch.dma("pool", wq[s][:], wq_d[:, hh * 128:(hh + 1) * 128].rearrange("(c p) n -> p c n", p=128), writes=[wqB[s]])
            for tb in range(NT):
                par = it % 2
                it += 1
                tsl = slice(tb * 512, (tb + 1) * 512)
                qb_ = par
                for c in range(DC):
                    sch.mm(banks[qb_][:, :], wq[s][:, c, :], actT[:, c, tsl], c == 0, c == DC - 1,
                           reads=[wqB[s]] + actTB[4 * tb:4 * tb + 4], writes=[bankB[qb_]])
                sch.copy("act", q2s[par][:], banks[qb_][:, :], reads=[bankB[qb_]], writes=[q2B[par]])
                for j in range(NMB):
                    sbk = 2 + j
                    sch.mm(banks[sbk][:, :], k2T[:, hh, j * 128:(j + 1) * 128], q2s[par][:], True, True,
                           reads=[kvB, q2B[par]], writes=[bankB[sbk]])
                    sch.act(pT[par][:, j, :], banks[sbk][:, :], AF.Exp, reads=[bankB[sbk]], writes=[pTB[par]], scale=SCALE)
                ob, db = (4, 5) if par == 0 else (6, 7)
                for j in range(NMB):
                    sch.mm(banks[ob][:, :], v2[:, j, hh * 128:(hh + 1) * 128], pT[par][:, j, :], j == 0, j == NMB - 1,
                           reads=[kvB, pTB[par]], writes=[bankB[ob]])
                for j in range(NMB):
                    sch.mm(banks[db][:, :], ones_b[:], pT[par][:, j, :], j == 0, j == NMB - 1,
                           reads=[pTB[par]], writes=[bankB[db]])
                sch.op("dve", (lambda r_=rec[par], d_=banks[db]: lambda e: e.reciprocal(out=r_[:], in_=d_[:, :]))(),
                       reads=[bankB[db]], writes=[recB[par]])
                sch.tt("dve", o2T[:, hh, tsl], banks[ob][:, :], rec[par][:], ALU.mult, reads=[bankB[ob], recB[par]],
                       writes=[o2B])
        sch.barrier()
        A.release(m0)
        return o2B

    o2B = stage_X()
    gemm_tm_res(o2T, X_HEADS, wo_d, x1_d, [[o2B], [sB(("x1", i)) for i in range(NB)]], acc_d, "acc")
    sch.barrier()
    A.release(base_mark)
    if cfg.stop_after == "XO":
        sch.emit(list(scrB.values()))
        return nc


    aff_all = A.alloc([128, NB, NEXP], F32, "aff_all")
    tv = A.alloc([128, NB, NEXP, 2], F32, "tv")
    idx_all = A.alloc([128, NEXP, 2], I32, "idx_all")
    NH = max(1, D // 2048) if "nh2" not in cfg.flags else 2
    CH = D // NH
    idx2_all = A.alloc([128, NEXP, 2, NH], I32, "idx2_all")
    acc_rows = acc_d.rearrange("s (h d) -> (s h) d", h=NH)
    val_all = A.alloc([128, NEXP, 2], F32, "val_all")
    iota_t = A.alloc([128, 256], F32, "iota")
    lt_t = A.alloc([128, 128], F32, "lt")
    tok_t = A.alloc([128, NB], F32, "tok")
    moe_mark = A.mark()
    affB = Buf("aff_all")
    tvB = Buf("tv")

    def stage_N3():
        m0 = A.mark()
        gbc = A.alloc([128, D], F32, "gbc")
        wr_sb = A.alloc([128, DC, NEXP], F32, "wr_sb")
        affT_sb = A.alloc([NEXP, S], F32, "affT_sb")
        ss = A.alloc([128, NB], F32, "ss3")
        sd = A.alloc([128, NB], F32, "sd3")
        rstd = A.alloc([128, NB], F32, "rstd3")
        mx = A.alloc([128, NB], F32, "mx")
        nmx = A.alloc([128, NB], F32, "nmx")
        sm = A.alloc([128, NB], F32, "sm")
        rs = A.alloc([128, NB], F32, "rs")
        ex = A.alloc([128, NB, NEXP], F32, "ex")
        xr = [A.alloc([128, D], F32, "xr3") for _ in range(2)]
        h3f = [A.alloc([128, D], F32, "h3f") for _ in range(2)]
        h3b = [A.alloc([128, D], BF16, "h3b") for _ in range(2)]
        h3T = [A.alloc([128, 4, 128], F32, "h3T") for _ in range(2)]
        xrB = [Buf("xr30"), Buf("xr31")]
        hfB = [Buf("h3f0"), Buf("h3f1")]
        hbB = [Buf("h3b0"), Buf("h3b1")]
        hTB_ = [Buf("h3T0"), Buf("h3T1")]
        cB = Buf("n3c")
        afTB = Buf("affT_sb")
        stB = [Buf("n3st%d" % i) for i in range(NB)]
        sch.dma("sp", gbc[:], gffn_d[0].partition_broadcast(128), writes=[cB])
        with nc.allow_non_contiguous_dma(reason="router weights are tiny"):
            pass
        sch.dma("sp", wr_sb[:], wr_d.rearrange("(c p) e -> p c e", p=128), writes=[cB])
        sch.dma("sp", iota_t[:], c_iota, writes=[cB])
        sch.dma("sp", lt_t[:], c_lt, writes=[cB])
        sch.dma("sp", tok_t[:], c_tok, writes=[cB])
        G4 = min(4, DC)
        step = 0
        for i in range(NB):
            s = i % 2
            sch.dma("sp", xr[s][:], acc_d[i * 128:(i + 1) * 128, :], reads=[sB(("acc", i))], writes=[xrB[s]])
            sch.op("dve", (lambda o_=h3b[s], x_=xr[s], i_=i: lambda e: e.scalar_tensor_tensor(
                out=o_[:], in0=x_[:], scalar=1.0, in1=x_[:], op0=ALU.mult, op1=ALU.mult,
                accum_out=ss[:, i_:i_ + 1]))(), reads=[xrB[s]], writes=[hbB[s], stB[i]])
            sch.act(sd[:, i:i + 1], ss[:, i:i + 1], AF.Sqrt, reads=[stB[i]], writes=[stB[i]], bias=eps_t[:], scale=1.0 / D)
            sch.op("dve", (lambda i_=i: lambda e: e.reciprocal(out=rstd[:, i_:i_ + 1], in_=sd[:, i_:i_ + 1]))(),
                   reads=[stB[i]], writes=[stB[i]])
            sch.op("dve", (lambda o_=h3f[s], x_=xr[s], i_=i: lambda e: e.scalar_tensor_tensor(
                out=o_[:], in0=x_[:], scalar=rstd[:, i_:i_ + 1], in1=gbc[:], op0=ALU.mult, op1=ALU.mult))(),
                reads=[xrB[s], stB[i], cB], writes=[hfB[s]], strict=[stB[i]])
            sch.copy("pool", h3b[s][:], h3f[s][:], reads=[hfB[s]], writes=[hbB[s]])
            sch.dma("sp", h3_d[i * 128:(i + 1) * 128, :], h3b[s][:], reads=[hbB[s]], writes=[sB(("h3", i))])
            for c0 in range(0, DC, G4):
                pb = step % 2
                k = step % 2
                step += 1
                for j in range(G4):
                    c = c0 + j
                    sch.tr(banks[pb][:, j * 128:(j + 1) * 128], h3f[s][:, c * 128:(c + 1) * 128], ident_f[:],
                           reads=[hfB[s]], writes=[bankB[pb]])
                ev = "act" if step % 2 == 0 else "dve"
                sch.copy(ev, h3T[k][:, 0:G4, :], banks[pb][:, 0:G4 * 128].rearrange("p (j t) -> p j t", t=128),
                         reads=[bankB[pb]], writes=[hTB_[k]])
                for j in range(G4):
                    c = c0 + j
                    sch.mm(banks[2][:, 0:NEXP], h3T[k][:, j, :], wr_sb[:, c, :], c == 0, c == DC - 1,
                           reads=[hTB_[k], cB], writes=[bankB[2]])
            sch.op("dve", (lambda i_=i: lambda e: e.reduce_max(out=mx[:, i_:i_ + 1], in_=banks[2][:, 0:NEXP], axis=AX.X))(),
                   reads=[bankB[2]], writes=[stB[i]])
            sch.ts("dve", nmx[:, i:i + 1], mx[:, i:i + 1], -1.0, None, ALU.mult, reads=[], writes=[stB[i]], strict=[stB[i]])
            sch.act(ex[:, i, :], banks[2][:, 0:NEXP], AF.Exp, reads=[bankB[2], stB[i]], writes=[stB[i]],
                    bias=nmx[:, i:i + 1], accum_out=sm[:, i:i + 1])
            sch.op("dve", (lambda i_=i: lambda e: e.reciprocal(out=rs[:, i_:i_ + 1], in_=sm[:, i_:i_ + 1]))(),
                   reads=[stB[i]], writes=[stB[i]])
            sch.ts("dve", aff_all[:, i, :], ex[:, i, :], rs[:, i:i + 1], None, ALU.mult, reads=[stB[i]], writes=[affB], strict=[stB[i]])
            sch.tr(banks[3][0:NEXP, 0:128], aff_all[:, i, :], ident_f[:], reads=[affB], writes=[bankB[3]])
            sch.copy("act", affT_sb[:, i * 128:(i + 1) * 128], banks[3][0:NEXP, 0:128], reads=[bankB[3]], writes=[afTB])
        sch.dma("sp", affT_d, affT_sb[:], reads=[afTB], writes=[sB("affT")])
        sch.copy("dve", tv[:, :, :, 0], tok_t[:].unsqueeze(2).to_broadcast([128, NB, NEXP]), reads=[cB], writes=[tvB])
        sch.copy("dve", tv[:, :, :, 1], aff_all[:], reads=[affB], writes=[tvB])
        sch.barrier()
        A.release(m0)

    stage_N3()
    if cfg.stop_after == "N3":
        sch.emit(list(scrB.values()))
        return nc

    CAP = cfg.CAP
    FC, DE = cfg.FC, cfg.DE
    assert CAP == 256
    accB = Buf("acc_scatter")

    def stage_RE():
        m0 = A.mark()
        bc = [A.alloc([128, S], F32, "bc") for _ in range(2)]
        bcB = [Buf("bc0"), Buf("bc1")]
        junk = A.alloc([128, S], BF16, "junk")
        rank = [A.alloc([128, NB], F32, "rank") for _ in range(2)]
        rk4 = [A.alloc([128, 4, NB], F32, "rk4") for _ in range(2)]
        rankB = [Buf("rank0"), Buf("rank1")]
        P = [A.alloc([128, CAP], F32, "P") for _ in range(4)]
        PB = [Buf("P%d" % i) for i in range(4)]
        idxf = A.alloc([128, 2, 2], F32, "idxf")
        idxB = Buf("idx_all")
        xg = [A.alloc([128, D], BF16, "xg") for _ in range(2)]
        xgB = [Buf("xg0"), Buf("xg1")]
        xgT = A.alloc([128, DC, CAP], BF16, "xgT")
        xgTB = Buf("xgT")
        actT_ = A.alloc([128, FC, CAP], BF16, "eact")
        eaB = Buf("eact")
        sa = [A.alloc([128, 4, CAP], BF16, "sa") for _ in range(2)]
        saB = [Buf("sa0"), Buf("sa1")]
        wgu = [A.alloc([128, DC, min(512, DE)], BF16, "wgu") for _ in range(2)]
        wguB = [Buf("wgu0"), Buf("wgu1")]
        wd = [A.alloc([128, FC, 512], BF16, "wd") for _ in range(2)]
        wdB = [Buf("wd0"), Buf("wd1")]
        ysb = [A.alloc([128, D], F32, "ysb") for _ in range(2)]
        ysB = [Buf("ys0"), Buf("ys1")]
        G8 = min(8, DC)
        cntr = {"p": 0, "tp": 0, "gu": 0, "gb": 0}

        def route_steps(e):
            s = e % 2
            steps = []

            def start():
                sch.dma("sp", bc[s][:], affT_d[e].partition_broadcast(128), reads=[sB("affT")], writes=[bcB[s]])
                sch.op("dve", (lambda r_=rk4[s]: lambda e_: e_.memset(r_[:], 0.0))(), writes=[rankB[s]])
            steps.append(start)
            for i in range(NB):
                steps.append((lambda i_=i: lambda: rank_block(e, s, i_))())
            steps.append(lambda: route_finish(e, s))
            return steps

        def rank_block(e, s, i):
            if True:
                a_t = aff_all[:, i, e:e + 1]
                lo, hi = i * 128, (i + 1) * 128
                if i > 0:
                    sch.ts("dve", junk[:, 0:lo], bc[s][:, 0:lo], a_t, 0.0, ALU.is_ge, ALU.add,
                           reads=[bcB[s], affB], writes=[rankB[s]], accum_out=rk4[s][:, 0, i:i + 1])
                if i < NB - 1:
                    sch.ts("dve", junk[:, hi:S], bc[s][:, hi:S], a_t, 0.0, ALU.is_gt, ALU.add,
                           reads=[bcB[s], affB], writes=[rankB[s]], accum_out=rk4[s][:, 1, i:i + 1])
                sch.ts("dve", junk[:, lo:hi], bc[s][:, lo:hi], a_t, 0.0, ALU.is_gt, ALU.add,
                       reads=[bcB[s], affB], writes=[rankB[s]], accum_out=rk4[s][:, 2, i:i + 1])
                sch.op("dve", (lambda b_=bc[s], a_=a_t, r_=rk4[s], i_=i, lo_=lo, hi_=hi: lambda e_: e_.scalar_tensor_tensor(
                    out=junk[:, lo_:hi_], in0=b_[:, lo_:hi_], scalar=a_, in1=lt_t[:], op0=ALU.is_equal, op1=ALU.mult,
                    accum_out=r_[:, 3, i_:i_ + 1]))(), reads=[bcB[s], affB], writes=[rankB[s]])
        def route_finish(e, s):
            sch.tt("dve", rank[s][:], rk4[s][:, 0, :], rk4[s][:, 1, :], ALU.add, reads=[], writes=[rankB[s]])
            sch.tt("dve", rank[s][:], rank[s][:], rk4[s][:, 2, :], ALU.add, reads=[], writes=[rankB[s]])
            sch.tt("dve", rank[s][:], rank[s][:], rk4[s][:, 3, :], ALU.add, reads=[], writes=[rankB[s]])
            for i in range(NB):
                k = cntr["p"] % 4
                cntr["p"] += 1
                sch.ts("pool", P[k][:], iota_t[:], rank[s][:, i:i + 1], None, ALU.is_equal,
                       reads=[rankB[s]], writes=[PB[k]])
                for hf in range(2):
                    sch.mm(banks[6 + hf][:, 0:2], P[k][:, hf * 128:(hf + 1) * 128], tv[:, i, e, :], i == 0, i == NB - 1,
                           reads=[PB[k], tvB], writes=[bankB[6 + hf]])
            for hf in range(2):
                sch.copy("dve", idxf[:, hf, :], banks[6 + hf][:, 0:2], reads=[bankB[6 + hf]], writes=[idxB])
            sch.copy("dve", idx_all[:, e, :], idxf[:, :, 0], reads=[], writes=[idxB])
            for hc in range(NH):
                sch.ts("dve", idx2_all[:, e, :, hc], idxf[:, :, 0], float(NH), float(hc), ALU.mult, ALU.add,
                       reads=[], writes=[idxB])
            sch.copy("dve", val_all[:, e, :], idxf[:, :, 1], reads=[], writes=[idxB])

        def expert(e, hook):
            for hf in range(2):
                sch.op("pool", (lambda o_=xg[hf], e_=e, hf_=hf: lambda eng: eng.indirect_dma_start(
                    out=o_[:], out_offset=None, in_=h3_d[:, :],
                    in_offset=bass.IndirectOffsetOnAxis(ap=idx_all[:, e_, hf_:hf_ + 1], axis=0)))(),
                    reads=[idxB] + [sB(("h3", i)) for i in range(NB)], writes=[xgB[hf]], dma=True)
                for c0 in range(0, DC, G8):
                    pb = cntr["tp"] % 2
                    cntr["tp"] += 1
                    pv = pbf(pb).rearrange("p (j t) -> p j t", t=128)
                    for j in range(G8):
                        c = c0 + j
                        sch.tr(pv[:, j, :], xg[hf][:, c * 128:(c + 1) * 128], ident_b[:], reads=[xgB[hf]],
                               writes=[bankB[pb]])
                    ev = "act" if cntr["tp"] % 2 == 0 else "dve"
                    sch.copy(ev, xgT[:, c0:c0 + G8, hf * 128:(hf + 1) * 128], pv[:, 0:G8, :], reads=[bankB[pb]],
                             writes=[xgTB])
            WBk = min(512, DE)
            NF4 = WBk // 128
            for blk in range(DE // WBk):
                for which in range(2):
                    sl = cntr["gu"] % 2
                    cntr["gu"] += 1
                    wsrc = wg_d if which == 0 else wu_d
                    sch.dma("pool", wgu[sl][:], wsrc[e][:, blk * WBk:(blk + 1) * WBk].rearrange("(c p) n -> p c n", p=128),
                            writes=[wguB[sl]])
                    for f4 in range(NF4):
                        fc = blk * NF4 + f4
                        pb = 2 + cntr["gb"] % 4
                        cntr["gb"] += 1
                        for c in range(DC):
                            sch.mm(banks[pb][:, 0:CAP], wgu[sl][:, c, f4 * 128:(f4 + 1) * 128], xgT[:, c, :], c == 0, c == DC - 1,
                                   reads=[wguB[sl], xgTB], writes=[bankB[pb]])
                        if which == 0:
                            sch.act(sa[blk % 2][:, f4, :], banks[pb][:, 0:CAP], AF.Silu, reads=[bankB[pb]], writes=[saB[blk % 2]])
                        else:
                            sch.tt("dve", actT_[:, fc, :], banks[pb][:, 0:CAP], sa[blk % 2][:, f4, :], ALU.mult,
                                   reads=[bankB[pb], saB[blk % 2]], writes=[eaB])
                        hook()
            for cb in range(D // 512):
                s = cb % 2
                sch.dma("pool", wd[s][:], wd_d[e][:, cb * 512:(cb + 1) * 512].rearrange("(f p) n -> p f n", p=128),
                        writes=[wdB[s]])
                for hf in range(2):
                    pb = 2 + (cb * 2 + hf) % 4
                    for fc in range(FC):
                        sch.mm(banks[pb][:, :], actT_[:, fc, hf * 128:(hf + 1) * 128], wd[s][:, fc, :], fc == 0, fc == FC - 1,
                               reads=[wdB[s], eaB], writes=[bankB[pb]])
                    sch.act(ysb[hf][:, cb * 512:(cb + 1) * 512], banks[pb][:, :], AF.Copy, reads=[bankB[pb], idxB],
                            writes=[ysB[hf]], scale=val_all[:, e, hf:hf + 1])
                hook()
            for hf in range(2):
                for hc in range(NH):
                    sch.op("pool", (lambda y_=ysb[hf], e_=e, hf_=hf, hc_=hc: lambda eng: eng.indirect_dma_start(
                        out=acc_rows[:, :],
                        out_offset=bass.IndirectOffsetOnAxis(ap=idx2_all[:, e_, hf_, hc_:hc_ + 1], axis=0),
                        in_=y_[:, hc_ * CH:(hc_ + 1) * CH], in_offset=None, compute_op=ALU.add))(),
                        reads=[ysB[hf], idxB], writes=[accB], dma=True)

        for st in route_steps(0):
            st()
        for e in range(NEXP):
            pending = route_steps(e + 1) if e + 1 < NEXP else []

            def hook():
                if pending:
                    pending.pop(0)()
            expert(e, hook)
            while pending:
                pending.pop(0)()
        sch.barrier()
        A.release(m0)

    stage_RE()
    if dbg:
        idx_dbg = dscr("s_idx", [128, NEXP, 2], I32)
        val_dbg = dscr("s_val", [128, NEXP, 2], F32)
        dB2 = sB("dbg_idx")
        sch.dma("sp", idx_dbg, idx_all[:], writes=[dB2])
        sch.dma("sp", val_dbg, val_all[:], writes=[dB2])
    A.release(base_mark)
    if cfg.stop_after == "E":
        sch.emit(list(scrB.values()) + [accB])
        return nc

    outB = []

    def stage_F():
        gbc = A.alloc([128, D], F32, "gfin")
        ss = A.alloc([128, NB], F32, "ssf")
        sd = A.alloc([128, NB], F32, "sdf")
        rstd = A.alloc([128, NB], F32, "rstdf")
        xr = [A.alloc([128, D], F32, "xrf") for _ in range(2)]
        oo = [A.alloc([128, D], F32, "oof") for _ in range(2)]
        jk = A.alloc([128, D], BF16, "jkf")
        xrB = [Buf("xrf0"), Buf("xrf1")]
        ooB = [Buf("oof0"), Buf("oof1")]
        jB = Buf("jkf")
        cB = Buf("fc")
        stB = [Buf("fst%d" % i) for i in range(NB)]
        sch.dma("sp", gbc[:], gfin_d[0].partition_broadcast(128), writes=[cB])
        for i in range(NB):
            s = i % 2
            sch.dma("sp", xr[s][:], acc_d[i * 128:(i + 1) * 128, :], reads=[accB, sB(("acc", i))], writes=[xrB[s]])
            sch.op("dve", (lambda x_=xr[s], i_=i: lambda e: e.scalar_tensor_tensor(
                out=jk[:], in0=x_[:], scalar=1.0, in1=x_[:], op0=ALU.mult, op1=ALU.mult,
                accum_out=ss[:, i_:i_ + 1]))(), reads=[xrB[s]], writes=[jB, stB[i]])
            sch.act(sd[:, i:i + 1], ss[:, i:i + 1], AF.Sqrt, reads=[stB[i]], writes=[stB[i]], bias=eps_t[:], scale=1.0 / D)
            sch.op("dve", (lambda i_=i: lambda e: e.reciprocal(out=rstd[:, i_:i_ + 1], in_=sd[:, i_:i_ + 1]))(),
                   reads=[stB[i]], writes=[stB[i]])
            sch.op("dve", (lambda o_=oo[s], x_=xr[s], i_=i: lambda e: e.scalar_tensor_tensor(
                out=o_[:], in0=x_[:], scalar=rstd[:, i_:i_ + 1], in1=gbc[:], op0=ALU.mult, op1=ALU.mult))(),
                reads=[xrB[s], stB[i], cB], writes=[ooB[s]], strict=[stB[i]])
            ob = Buf("out%d" % i)
            outB.append(ob)
            sch.dma("sp", out_d[i * 128:(i + 1) * 128, :], oo[s][:], reads=[ooB[s]], writes=[ob])

    stage_F()
    sch.emit(outB)
    return nc

    final_reads = []
    if cfg.stop_after == "P1":
        sch.emit(final_reads + list(scrB.values()))
        return nc

    sch.emit(final_reads)
    return nc


def prep_core_inputs(cfg, inp, b, consts):
    D, DC = cfg.D, cfg.DC
    f = lambda a: np.ascontiguousarray(np.asarray(a, dtype=np.float32))
    dri, dci, col_ok, drv = _na_index_tables()
    rpb = f(inp["rpb"])[0]
    rpbg = np.ascontiguousarray(rpb[:, dri, dci[None, :, :]])
    m = {
        "x": f(inp["x"][b]),
        "mem": f(inp["mem"][b]),
        "gmix": col_layout(f(inp["norm_mix"])[0], DC),
        "gcross": col_layout(f(inp["norm_cross"])[0], DC),
        "gmem": col_layout(f(inp["norm_mem"])[0], DC),
        "gffn": f(inp["norm_ffn"])[0][None, :],
        "gfin": f(inp["norm_final"])[None, :],
        "w_in": f(inp["w_in"])[0],
        "bgate": col_layout(f(inp["b_gate"])[0], 2 * DC),
        "sink": f(inp["sink"])[0][None, :],
        "rpbg": rpbg,
        "w_branch_a": f(inp["w_branch_a"])[0],
        "w_branch_b": f(inp["w_branch_b"])[0],
        "w_out": f(inp["w_out"])[0],
        "wq_x": f(inp["wq_x"])[0],
        "wk_x": f(inp["wk_x"])[0],
        "wv_x": f(inp["wv_x"])[0],
        "wo_x": f(inp["wo_x"])[0],
        "w_router": f(inp["w_router"])[0],
        "w_gate": f(inp["w_gate"])[0],
        "w_up": f(inp["w_up"])[0],
        "w_down": f(inp["w_down"])[0],
        "c_ident": consts["ident_f"],
        "c_maskL": consts["maskL"],
        "c_maskR": consts["maskR"],
        "c_namask": consts["namask"],
        "c_cos": consts["cos"],
        "c_sin": consts["sin"],
        "c_rotR": consts["rotR"],
        "c_iota": consts["iota"],
        "c_lt": consts["lt"],
        "c_tok": consts["tokidx"],
    }
    return m


def kernel(**inputs):
    x = np.asarray(inputs["x"])
    B, S, D = x.shape
    cfg = Cfg(D=D, S=S, MEM=np.asarray(inputs["mem"]).shape[1])
    consts = make_consts(S)
    nc = build(cfg)
    in_maps = [prep_core_inputs(cfg, inputs, b, consts) for b in range(B)]
    res = run_bass_kernel_spmd(nc, in_maps, core_ids=list(range(B)))
    return np.stack([np.asarray(r["out"], dtype=np.float32) for r in res.results], axis=0)
```
